# Optimizing a Trainium2 kernel written in Bass

```python
import jax, jax.numpy as jnp
from jax import lax
import numpy as np

D_MODEL = 2048
BATCH = 4
SEQ = 4096
DEPTH = 4

HEAD_DIM = 128
FOX_HEADS = D_MODEL // (2 * HEAD_DIM)
SWA_Q_HEADS = D_MODEL // (2 * HEAD_DIM)
SWA_KV_HEADS = 2
SWA_GROUP = SWA_Q_HEADS // SWA_KV_HEADS
SWA_WINDOW = 128
Q_BLOCK = 128
ROPE_THETA = 10000.0
MLSTM_HEADS = 8
MLSTM_QK_DIM = D_MODEL // (2 * MLSTM_HEADS)
MLSTM_V_DIM = D_MODEL // MLSTM_HEADS
MLSTM_CHUNK = 64
D_FF = 5632
CONV_WIDTH = 3
LN_EPS = 1e-5
DEEPNORM_ALPHA = (2 * DEPTH) ** 0.25
DEEPNORM_BETA = (8 * DEPTH) ** -0.25
FORGET_BIAS_INIT = 3.0
N_EVEN = (DEPTH + 1) // 2
N_ODD = DEPTH // 2

FOX_DIM = FOX_HEADS * HEAD_DIM
SWA_Q_DIM = SWA_Q_HEADS * HEAD_DIM
SWA_KV_DIM = SWA_KV_HEADS * HEAD_DIM
ATTN_SPLIT_SIZES = (FOX_DIM, FOX_DIM, FOX_DIM, FOX_HEADS, SWA_Q_DIM, SWA_KV_DIM, SWA_KV_DIM)
ATTN_IN_DIM = 3 * FOX_DIM + FOX_HEADS + SWA_Q_DIM + 2 * SWA_KV_DIM
FOX_F_OFF = 3 * FOX_DIM
ATTN_OUT_DIM = FOX_DIM + SWA_Q_DIM

MLSTM_QK_TOT = MLSTM_HEADS * MLSTM_QK_DIM
MLSTM_V_TOT = MLSTM_HEADS * MLSTM_V_DIM
MLSTM_SPLIT_SIZES = (MLSTM_QK_TOT, MLSTM_QK_TOT, MLSTM_V_TOT, MLSTM_V_TOT, MLSTM_HEADS, MLSTM_HEADS)
MLSTM_IN_DIM = 2 * MLSTM_QK_TOT + 2 * MLSTM_V_TOT + 2 * MLSTM_HEADS
MLSTM_F_OFF = 2 * MLSTM_QK_TOT + 2 * MLSTM_V_TOT + MLSTM_HEADS

kernel_name = "fox_swa_sink_mlstm_convffn_deepnorm_hybrid"


def split_columns(a, sizes):
    out, off = [], 0
    for s in sizes:
        out.append(a[..., off:off + s])
        off += s
    return out


def layer_norm(x, g, b):
    xf = x.astype(jnp.float32)
    mu = jnp.mean(xf, axis=-1, keepdims=True)
    var = jnp.mean(jnp.square(xf - mu), axis=-1, keepdims=True)
    y = (xf - mu) * lax.rsqrt(var + LN_EPS)
    return (y * g.astype(jnp.float32) + b.astype(jnp.float32)).astype(x.dtype)


def apply_rope(x):
    S = x.shape[1]
    half = HEAD_DIM // 2
    inv_freq = jnp.power(ROPE_THETA, -jnp.arange(half, dtype=jnp.float32) * (2.0 / HEAD_DIM))
    ang = jnp.arange(S, dtype=jnp.float32)[:, None] * inv_freq[None, :]
    cos = jnp.cos(ang)[None, :, None, :]
    sin = jnp.sin(ang)[None, :, None, :]
    xf = x.astype(jnp.float32)
    x1, x2 = xf[..., :half], xf[..., half:]
    return jnp.concatenate([x1 * cos - x2 * sin, x2 * cos + x1 * sin], axis=-1).astype(x.dtype)


def fox_attention(q, k, v, log_f):
    B, S, H, d = q.shape
    nb = S // Q_BLOCK
    scale = d ** -0.5
    c = jnp.cumsum(log_f, axis=1)
    c_k = jnp.transpose(c, (0, 2, 1))
    q_blocks = jnp.transpose(q.reshape(B, nb, Q_BLOCK, H, d), (1, 0, 2, 3, 4))
    c_blocks = jnp.transpose(c.reshape(B, nb, Q_BLOCK, H), (1, 0, 3, 2))
    k_pos = jnp.arange(S)

    def one_block(args):
        qi, cqi, i = args
        s = jnp.einsum('bqhd,bkhd->bhqk', qi, k, preferred_element_type=jnp.float32) * scale
        s = s + (cqi[..., :, None] - c_k[:, :, None, :])
        q_pos = i * Q_BLOCK + jnp.arange(Q_BLOCK)
        causal = k_pos[None, :] <= q_pos[:, None]
        s = jnp.where(causal[None, None], s, -jnp.inf)
        p = jax.nn.softmax(s, axis=-1)
        o = jnp.einsum('bhqk,bkhd->bqhd', p.astype(v.dtype), v, preferred_element_type=jnp.float32)
        return o.astype(q.dtype)

    out = lax.map(one_block, (q_blocks, c_blocks, jnp.arange(nb)))
    return jnp.transpose(out, (1, 0, 2, 3, 4)).reshape(B, S, H * d)


def swa_sink_attention(q, k, v, sinks):
    B, S, Hq, d = q.shape
    W = SWA_WINDOW
    nb = S // W
    scale = d ** -0.5
    qb = q.reshape(B, nb, W, SWA_KV_HEADS, SWA_GROUP, d)
    kb = k.reshape(B, nb, W, SWA_KV_HEADS, d)
    vb = v.reshape(B, nb, W, SWA_KV_HEADS, d)
    k_prev = jnp.concatenate([jnp.zeros_like(kb[:, :1]), kb[:, :-1]], axis=1)
    v_prev = jnp.concatenate([jnp.zeros_like(vb[:, :1]), vb[:, :-1]], axis=1)
    k_win = jnp.concatenate([k_prev, kb], axis=2)
    v_win = jnp.concatenate([v_prev, vb], axis=2)
    s = jnp.einsum('bnqkgd,bnskd->bnkgqs', qb, k_win, preferred_element_type=jnp.float32) * scale
    blk = jnp.arange(nb)[:, None, None]
    q_pos = blk * W + jnp.arange(W)[None, :, None]
    k_pos = (blk - 1) * W + jnp.arange(2 * W)[None, None, :]
    valid = (k_pos <= q_pos) & (k_pos > q_pos - W) & (k_pos >= 0)
    s = jnp.where(valid[None, :, None, None], s, -jnp.inf)
    sink = sinks.astype(jnp.float32).reshape(SWA_KV_HEADS, SWA_GROUP)[None, None, :, :, None, None]
    m = jnp.maximum(jnp.max(s, axis=-1, keepdims=True), sink)
    p = jnp.exp(s - m)
    denom = jnp.sum(p, axis=-1, keepdims=True) + jnp.exp(sink - m)
    o = jnp.einsum('bnkgqs,bnskd->bnqkgd', (p / denom).astype(v.dtype), v_win,
                   preferred_element_type=jnp.float32)
    return o.astype(q.dtype).reshape(B, S, Hq * d)


def mlstm_chunkwise(q, k, v, i_pre, f_pre):
    B, S, H, dk = q.shape
    dv = v.shape[-1]
    L = MLSTM_CHUNK
    nc = S // L
    qf = q.astype(jnp.float32)
    kf = k.astype(jnp.float32) * (dk ** -0.5)
    vf = v.astype(jnp.float32)
    log_f = jax.nn.log_sigmoid(f_pre)

    def to_chunks(a):
        return jnp.transpose(a.reshape(B, nc, L, H, a.shape[-1]), (1, 0, 3, 2, 4))

    def gate_chunks(a):
        return jnp.transpose(a.reshape(B, nc, L, H), (1, 0, 3, 2))

    qc, kc, vc = to_chunks(qf), to_chunks(kf), to_chunks(vf)
    ic = gate_chunks(i_pre)
    b = jnp.cumsum(gate_chunks(log_f), axis=-1)
    b_last = b[..., -1]
    tri = jnp.arange(L)[:, None] >= jnp.arange(L)[None, :]
    d_log = jnp.where(tri, b[..., :, None] - b[..., None, :] + ic[..., None, :], -jnp.inf)
    m_intra = jnp.max(d_log, axis=-1)
    g = b_last[..., None] - b + ic

    def step(carry, inp):
        Cs, ns, m = carry
        qx, kx, vx, bx, dx, mi, gx, bl = inp
        m_inter = bx + m[..., None]
        m_t = jnp.maximum(m_inter, mi)
        inter = jnp.exp(m_inter - m_t)
        w = jnp.exp(dx - m_t[..., None])
        sm = w * jnp.einsum('bhtd,bhsd->bhts', qx, kx)
        num = inter[..., None] * jnp.einsum('bhvd,bhtd->bhtv', Cs, qx) + jnp.einsum('bhts,bhsv->bhtv', sm, vx)
        dot = inter * jnp.einsum('bhd,bhtd->bht', ns, qx) + jnp.sum(sm, axis=-1)
        den = jnp.maximum(jnp.abs(dot), jnp.exp(-m_t))
        h = num / den[..., None]
        m_new = jnp.maximum(bl + m, jnp.max(gx, axis=-1))
        decay = jnp.exp(bl + m - m_new)
        wg = jnp.exp(gx - m_new[..., None])
        Cs_new = decay[..., None, None] * Cs + jnp.einsum('bhsv,bhsd->bhvd', vx * wg[..., None], kx)
        ns_new = decay[..., None] * ns + jnp.einsum('bhs,bhsd->bhd', wg, kx)
        return (Cs_new, ns_new, m_new), h

    init = (jnp.zeros((B, H, dv, dk), jnp.float32), jnp.zeros((B, H, dk), jnp.float32),
            jnp.zeros((B, H), jnp.float32))
    _, hs = lax.scan(step, init, (qc, kc, vc, b, d_log, m_intra, g, b_last))
    return jnp.transpose(hs, (1, 0, 3, 2, 4)).reshape(B, S, H * dv)


def attention_mixer(x, w_in, b_in, sinks, w_out):
    B, S, _ = x.shape
    proj = jnp.einsum('bsd,de->bse', x, w_in) + b_in
    fq, fk, fv, ff, sq, sk, sv = split_columns(proj, ATTN_SPLIT_SIZES)
    fox_out = fox_attention(fq.reshape(B, S, FOX_HEADS, HEAD_DIM), fk.reshape(B, S, FOX_HEADS, HEAD_DIM),
                            fv.reshape(B, S, FOX_HEADS, HEAD_DIM),
                            jax.nn.log_sigmoid(ff.astype(jnp.float32)))
    swa_out = swa_sink_attention(apply_rope(sq.reshape(B, S, SWA_Q_HEADS, HEAD_DIM)),
                                 apply_rope(sk.reshape(B, S, SWA_KV_HEADS, HEAD_DIM)),
                                 sv.reshape(B, S, SWA_KV_HEADS, HEAD_DIM), sinks)
    heads = jnp.concatenate([fox_out, swa_out], axis=-1)
    return jnp.einsum('bse,ed->bsd', heads, w_out)


def mlstm_mixer(x, w_in, b_in, w_out):
    B, S, _ = x.shape
    proj = jnp.einsum('bsd,de->bse', x, w_in) + b_in
    q, k, v, o, ig, fg = split_columns(proj, MLSTM_SPLIT_SIZES)
    h = mlstm_chunkwise(q.reshape(B, S, MLSTM_HEADS, MLSTM_QK_DIM), k.reshape(B, S, MLSTM_HEADS, MLSTM_QK_DIM),
                        v.reshape(B, S, MLSTM_HEADS, MLSTM_V_DIM), ig.astype(jnp.float32), fg.astype(jnp.float32))
    h = (jax.nn.sigmoid(o.astype(jnp.float32)) * h).astype(x.dtype)
    return jnp.einsum('bse,ed->bsd', h, w_out)


def conv_ffn(x, w_up, conv_w, conv_b, w_down):
    S = x.shape[1]
    u = jnp.einsum('bsd,df->bsf', x, w_up)
    u_pad = jnp.pad(u, ((0, 0), (CONV_WIDTH - 1, 0), (0, 0)))
    c = conv_b + sum(conv_w[j] * u_pad[:, j:j + S] for j in range(CONV_WIDTH))
    gate, val = c[..., :D_FF], c[..., D_FF:]
    return jnp.einsum('bsf,fd->bsd', jax.nn.silu(gate) * val, w_down)


def setup_inputs(seed: int = 0) -> dict:
    key = jax.random.key(seed)
    ks = jax.random.split(key, 20)
    f32 = jnp.float32

    def nrm(k, shape, scale):
        return jax.random.normal(k, shape, f32) * scale

    x = nrm(ks[0], (BATCH, SEQ, D_MODEL), 1.0)
    attn_w_in = nrm(ks[1], (N_EVEN, D_MODEL, ATTN_IN_DIM), D_MODEL ** -0.5)
    attn_b_in = nrm(ks[2], (N_EVEN, ATTN_IN_DIM), 0.02).at[:, FOX_F_OFF:FOX_F_OFF + FOX_HEADS].add(FORGET_BIAS_INIT)
    attn_sinks = nrm(ks[3], (N_EVEN, SWA_Q_HEADS), 0.5)
    attn_w_out = nrm(ks[4], (N_EVEN, ATTN_OUT_DIM, D_MODEL), ATTN_OUT_DIM ** -0.5 * DEEPNORM_BETA)
    mlstm_w_in = nrm(ks[5], (N_ODD, D_MODEL, MLSTM_IN_DIM), D_MODEL ** -0.5)
    mlstm_b_in = nrm(ks[6], (N_ODD, MLSTM_IN_DIM), 0.02).at[:, MLSTM_F_OFF:MLSTM_F_OFF + MLSTM_HEADS].add(FORGET_BIAS_INIT)
    mlstm_w_out = nrm(ks[7], (N_ODD, MLSTM_V_TOT, D_MODEL), MLSTM_V_TOT ** -0.5 * DEEPNORM_BETA)
    ffn_w_up = nrm(ks[8], (DEPTH, D_MODEL, 2 * D_FF), D_MODEL ** -0.5)
    ffn_conv_w = nrm(ks[9], (DEPTH, CONV_WIDTH, 2 * D_FF), CONV_WIDTH ** -0.5)
    ffn_conv_b = nrm(ks[10], (DEPTH, 2 * D_FF), 0.02)
    ffn_w_down = nrm(ks[11], (DEPTH, D_FF, D_MODEL), D_FF ** -0.5 * DEEPNORM_BETA)
    ln1_g = 1.0 + nrm(ks[12], (DEPTH, D_MODEL), 0.02)
    ln1_b = nrm(ks[13], (DEPTH, D_MODEL), 0.02)
    ln2_g = 1.0 + nrm(ks[14], (DEPTH, D_MODEL), 0.02)
    ln2_b = nrm(ks[15], (DEPTH, D_MODEL), 0.02)
    return {"x": x, "attn_w_in": attn_w_in, "attn_b_in": attn_b_in, "attn_sinks": attn_sinks,
            "attn_w_out": attn_w_out, "mlstm_w_in": mlstm_w_in, "mlstm_b_in": mlstm_b_in,
            "mlstm_w_out": mlstm_w_out, "ffn_w_up": ffn_w_up, "ffn_conv_w": ffn_conv_w,
            "ffn_conv_b": ffn_conv_b, "ffn_w_down": ffn_w_down, "ln1_g": ln1_g, "ln1_b": ln1_b,
            "ln2_g": ln2_g, "ln2_b": ln2_b}


def reference(x, attn_w_in, attn_b_in, attn_sinks, attn_w_out, mlstm_w_in, mlstm_b_in, mlstm_w_out,
              ffn_w_up, ffn_conv_w, ffn_conv_b, ffn_w_down, ln1_g, ln1_b, ln2_g, ln2_b):
    for layer in range(DEPTH):
        j = layer // 2
        if layer % 2 == 0:
            y = attention_mixer(x, attn_w_in[j], attn_b_in[j], attn_sinks[j], attn_w_out[j])
        else:
            y = mlstm_mixer(x, mlstm_w_in[j], mlstm_b_in[j], mlstm_w_out[j])
        x = layer_norm(DEEPNORM_ALPHA * x + y, ln1_g[layer], ln1_b[layer])
        y = conv_ffn(x, ffn_w_up[layer], ffn_conv_w[layer], ffn_conv_b[layer], ffn_w_down[layer])
        x = layer_norm(DEEPNORM_ALPHA * x + y, ln2_g[layer], ln2_b[layer])
    return x
```

```python
import contextlib
import numpy as np
import concourse.bass as bass
import concourse.mybir as mybir
from concourse.bass_utils import run_bass_kernel_spmd

F32 = mybir.dt.float32
BF16 = mybir.dt.bfloat16
U8 = mybir.dt.uint8
ALU = mybir.AluOpType
AF = mybir.ActivationFunctionType
AX = mybir.AxisListType

ENGS = ["pe", "act", "dve", "pool", "sp"]

D = 2048
T = 4096
TH = 2048
NH = 2050
DFF = 5632
NJ = 44
ALPHA = float(8 ** 0.25)
EPS = 1e-5
SCALE = float(128 ** -0.5)


class Buf:
    __slots__ = ("name", "last_w", "readers")

    def __init__(self, name):
        self.name = name
        self.last_w = None
        self.readers = []


class Chan:
    __slots__ = ("name", "sem", "count", "last")

    def __init__(self, name):
        self.name = name
        self.sem = None
        self.count = 0
        self.last = None


class Op:
    __slots__ = ("eng", "fn", "deps", "idx", "chan", "chan_count", "waits", "marked", "count", "snap", "gid")


class Prog:
    def __init__(self, nc):
        self.nc = nc
        self.ops = []
        self.streams = {e: [] for e in ENGS}
        self.chans = []
        self.fence = None
        self.fence_done = set()

    def chan(self, name):
        c = Chan(name)
        self.chans.append(c)
        return c

    def barrier(self):
        f = []
        for e in ENGS:
            if self.streams[e]:
                f.append(self.streams[e][-1].gid)
        for c in self.chans:
            if c.last is not None:
                f.append(c.last)
        self.fence = f
        self.fence_done = set()

    def op(self, eng, fn, reads=(), writes=(), chan=None):
        o = Op()
        o.eng = eng
        o.fn = fn
        o.gid = len(self.ops)
        o.idx = len(self.streams[eng])
        o.chan = chan
        o.marked = False
        o.count = None
        o.waits = None
        o.snap = None
        o.chan_count = 0
        if chan is not None:
            chan.count += 1
            o.chan_count = chan.count
            chan.last = o.gid
        deps = set()
        for b in reads:
            if b.last_w is not None:
                deps.add(b.last_w)
        for b in writes:
            if b.last_w is not None:
                deps.add(b.last_w)
            deps.update(b.readers)
        if self.fence is not None and eng not in self.fence_done:
            deps.update(self.fence)
            self.fence_done.add(eng)
        deps.discard(o.gid)
        o.deps = deps
        for b in reads:
            b.readers.append(o.gid)
        for b in writes:
            b.last_w = o.gid
            b.readers = []
        self.ops.append(o)
        self.streams[eng].append(o)
        return o.gid

    def resolve(self):
        seen = {e: {} for e in ENGS}
        ops = self.ops
        for o in ops:
            e = o.eng
            sc = seen[e]
            need = {}
            for d in o.deps:
                y = ops[d]
                if y.chan is not None:
                    key = ("c", id(y.chan))
                    val = y.chan_count
                else:
                    if y.eng == e and e == "pe":
                        continue
                    key = ("e", y.eng)
                    val = y.idx + 1
                cur = need.get(key)
                if cur is None or cur[0] < val:
                    need[key] = (val, y)
            waits = []
            for key, (val, y) in need.items():
                if sc.get(key, 0) >= val:
                    continue
                waits.append((key, val, y))
            for key, val, y in waits:
                if y.snap is not None:
                    for k2, v2 in y.snap.items():
                        if sc.get(k2, 0) < v2:
                            sc[k2] = v2
                if sc.get(key, 0) < val:
                    sc[key] = val
                if key[0] == "e":
                    y.marked = True
            o.waits = waits
            snap = dict(sc)
            if o.chan is None:
                k = ("e", e)
                if snap.get(k, 0) < o.idx:
                    snap[k] = o.idx
            o.snap = snap
        for e in ENGS:
            c = 0
            for o in self.streams[e]:
                if o.marked:
                    c += 1
                    o.count = c
        for o in ops:
            o.snap = None

    def emit(self, final_chans=()):
        nc = self.nc
        self.resolve()
        with contextlib.ExitStack() as st:
            esem = {e: st.enter_context(nc.semaphore("es_" + e)) for e in ENGS if e != "sp"}
            for i, c in enumerate(self.chans):
                if c.count:
                    c.sem = st.enter_context(nc.semaphore("ch%d" % i))
            block = st.enter_context(nc.Block())

            def run_stream(ename, eng):
                for o in self.streams[ename]:
                    for key, val, y in o.waits:
                        if key[0] == "c":
                            eng.wait_ge(y.chan.sem, 16 * val)
                        else:
                            eng.wait_ge(esem[y.eng], y.count)
                    inst = o.fn(eng)
                    if o.chan is not None:
                        inst.then_inc(o.chan.sem, 16)
                    elif o.marked:
                        inst.then_inc(esem[ename], 1)
                if ename == "sp":
                    for c in final_chans:
                        if c.count:
                            eng.wait_ge(c.sem, 16 * c.count)

            @block.tensor
            def _(eng):
                run_stream("pe", eng)

            @block.scalar
            def _(eng):
                run_stream("act", eng)

            @block.vector
            def _(eng):
                run_stream("dve", eng)

            @block.gpsimd
            def _(eng):
                run_stream("pool", eng)

            @block.sync
            def _(eng):
                run_stream("sp", eng)


ARENA_BYTES = 200 * 1024
ESZ = {F32: 4, BF16: 2}


class Tile:
    __slots__ = ("ap", "buf", "chan")

    def __init__(self, ap, buf, chan=None):
        self.ap = ap
        self.buf = buf
        self.chan = chan


class Ctx:
    def __init__(self, nc, st):
        self.nc = nc
        self.P = Prog(nc)
        self.arena = st.enter_context(nc.sbuf_tensor("arena", [128, ARENA_BYTES], U8))
        self.off = 0
        self.banks = []
        for i in range(8):
            ps = st.enter_context(nc.psum_tensor("bank%d" % i, [128, 512], F32))
            self.banks.append(Tile(ps[:], Buf("bank%d" % i)))
        self.nt = 0
        self.persist = 0

    def reset(self):
        self.off = self.persist
        self.P.barrier()

    def tile(self, shape, dt, name=None, chan=False):
        p = shape[0]
        n = int(np.prod(shape[1:]))
        nbytes = n * ESZ[dt]
        off = (self.off + 63) // 64 * 64
        assert off + nbytes <= ARENA_BYTES, ("SBUF arena overflow", name, off, nbytes)
        self.off = off + nbytes
        ap = self.arena[0:p, off:off + nbytes].bitcast(dt)
        if len(shape) == 3:
            ap = ap.rearrange("p (a b) -> p a b", a=shape[1])
        elif len(shape) == 4:
            ap = ap.rearrange("p (a b c) -> p a b c", a=shape[1], b=shape[2])
        self.nt += 1
        nm = "%s_%d" % (name or "t", self.nt)
        return Tile(ap, Buf(nm), self.P.chan(nm) if chan else None)

    def ring(self, n, shape, dt, name, chan=False):
        return Ring([self.tile(shape, dt, name, chan) for _ in range(n)])


class Ring:
    def __init__(self, items):
        self.items = items
        self.i = 0

    def next(self):
        t = self.items[self.i % len(self.items)]
        self.i += 1
        return t


def bufs(*ts):
    return [t.buf for t in ts]


def op_dma(C, q, dst_ap, src_ap, reads, writes, chan, **kw):
    C.P.op(q, lambda e: e.dma_start(out=dst_ap, in_=src_ap, **kw), reads, writes, chan)


def op_mm(C, out_ap, lhsT, rhs, start, stop, reads, writes):
    C.P.op("pe", lambda e: e.matmul(out_ap, lhsT=lhsT, rhs=rhs, start=start, stop=stop), reads, writes)


def op_act(C, out_ap, in_ap, func, reads, writes, bias=None, scale=None):
    kw = {}
    if bias is not None:
        kw["bias"] = bias
    if scale is not None:
        kw["scale"] = scale
    C.P.op("act", lambda e: e.activation(out=out_ap, in_=in_ap, func=func, **kw), reads, writes)


def op_tt(C, eng, out_ap, in0, in1, op, reads, writes):
    C.P.op(eng, lambda e: e.tensor_tensor(out=out_ap, in0=in0, in1=in1, op=op), reads, writes)


def op_ts(C, eng, out_ap, in0, s1, s2, op0, op1, reads, writes):
    if op1 is None:
        C.P.op(eng, lambda e: e.tensor_scalar(out=out_ap, in0=in0, scalar1=s1, scalar2=None, op0=op0), reads, writes)
    else:
        C.P.op(eng, lambda e: e.tensor_scalar(out=out_ap, in0=in0, scalar1=s1, scalar2=s2, op0=op0, op1=op1), reads, writes)


def op_stt(C, out_ap, in0, scalar, in1, op0, op1, reads, writes):
    C.P.op("dve", lambda e: e.scalar_tensor_tensor(out=out_ap, in0=in0, scalar=scalar, in1=in1, op0=op0, op1=op1), reads, writes)


def op_copy(C, eng, out_ap, in_ap, reads, writes):
    if eng == "act":
        C.P.op("act", lambda e: e.copy(out=out_ap, in_=in_ap), reads, writes)
    else:
        C.P.op(eng, lambda e: e.tensor_copy(out=out_ap, in_=in_ap), reads, writes)


def op_memset(C, eng, ap, val, writes):
    C.P.op(eng, lambda e: e.memset(ap, val), (), writes)


CST_COLS = 1024


def make_consts():
    c = np.zeros((128, CST_COLS), np.float32)
    k = np.arange(128)
    c[:, 0:128] = (k[:, None] <= k[None, :]).astype(np.float32)
    c[127, 128:256] = 1.0
    c[:, 256:384] = np.eye(128, dtype=np.float32)
    r = np.zeros((128, 128), np.float32)
    for j in range(64):
        r[j + 64, j] = -1.0
        r[j, j + 64] = 1.0
    c[:, 384:512] = r
    for h in range(4):
        c[h, 512 + h * 128: 512 + (h + 1) * 128] = 1.0
    return c


def make_rope():
    half = 64
    inv = np.power(np.float32(10000.0), -np.arange(half, dtype=np.float32) * np.float32(2.0 / 128)).astype(np.float32)
    ang = np.arange(T, dtype=np.float32)[:, None] * inv[None, :]
    cs = np.cos(ang).astype(np.float32).T
    sn = np.sin(ang).astype(np.float32).T
    return np.concatenate([np.concatenate([cs, cs], 0), np.concatenate([sn, sn], 0)], axis=1)


class Consts:
    pass


def load_consts(C, cst_dram, need_rope, rope_dram=None):
    K = Consts()
    t = C.tile([128, CST_COLS], F32, "cst", chan=True)
    op_dma(C, "sp", t.ap, cst_dram, (), [t.buf], t.chan)
    K.t = t
    K.tri = t.ap[:, 0:128]
    K.e127 = t.ap[:, 128:256]
    K.ident = t.ap[:, 256:384]
    K.rmat = t.ap[:, 384:512]
    K.sel = [t.ap[0:4, 512 + h * 128: 512 + (h + 1) * 128] for h in range(4)]
    ob = C.tile([128, 128], BF16, "ones_bf")
    op_memset(C, "dve", ob.ap, 1.0, [ob.buf])
    K.ones_bf = ob
    if need_rope:
        rp = C.tile([128, 2 * T], F32, "rope", chan=True)
        op_dma(C, "sp", rp.ap, rope_dram, (), [rp.buf], rp.chan)
        K.rope = rp
    C.persist = C.off
    return K


def phase_A1(C, K, lay, xsrc, x_f32, w_dram, bfm_dram, btm_dram, dst):
    P = C.P
    C.reset()
    ncols = lay["ncols"]
    W = C.tile([128, 16, ncols], BF16, "W", chan=True)
    op_dma(C, "pool", W.ap.rearrange("p a b -> p (a b)"), w_dram.rearrange("p a b -> p (a b)"), (), [W.buf], W.chan,
           max_dma_last_dim=4096)
    nfm = lay["nfm"]
    bfm = C.tile([128, nfm], F32, "bfm", chan=True)
    op_dma(C, "sp", bfm.ap, bfm_dram, (), [bfm.buf], bfm.chan)
    ntm = lay["ntm"]
    btm = C.tile([1, ntm], BF16, "btm", chan=True)
    op_dma(C, "pool", btm.ap, btm_dram, (), [btm.buf], btm.chan)
    ones_row = K.ones_bf.ap[0:1, :]
    xring = C.ring(2, [128, 16, 512], BF16, "xT", chan=True)
    oring = C.ring(3, [128, 512], BF16, "ofm", chan=True)
    gring = C.ring(2, [8, 512], F32, "ogate", chan=True)
    tring = [C.ring(2, [128, 4, n], BF16, "otm%d" % gi, chan=True) for gi, (c0, n) in enumerate(lay["tm_groups"])]
    qfring = C.ring(2, [128, 512], F32, "qf")
    t1ring = C.ring(2, [128, 512], F32, "t1")
    t2ring = C.ring(2, [128, 512], F32, "t2")
    bring = Ring(C.banks[0:6])
    rring = Ring(C.banks[6:8])

    def load_x(tt):
        xt = xring.next()
        r, t0 = tt // 4, (tt % 4) * 512
        src = xsrc[r, :, t0:t0 + 512].rearrange("(c p) t -> p c t", p=128)
        if x_f32:
            op_dma(C, "pool", xt.ap, src, (), [xt.buf], xt.chan, max_dma_last_dim=2048)
        else:
            op_dma(C, "sp", xt.ap, src, (), [xt.buf], xt.chan)
        return xt

    xt_next = load_x(0)
    for tt in range(8):
        xt = xt_next
        if tt + 1 < 8:
            xt_next = load_x(tt + 1)
        tok0 = tt * 512
        for ci in range(nfm):
            kind = lay["kinds"][ci]
            M = 128 if kind != "gate" else lay["gate_m"]
            c0 = ci * 128
            bk = bring.next()
            for kc in range(16):
                op_mm(C, bk.ap[0:M, :], W.ap[:, kc, c0:c0 + M], xt.ap[:, kc, :], kc == 0, kc == 15,
                      bufs(W, xt), [bk.buf])
            bias = bfm.ap[0:M, ci:ci + 1]
            if kind == "gate":
                og = gring.next()
                op_act(C, og.ap[0:M, :], bk.ap[0:M, :], AF.Identity, bufs(bk, bfm), [og.buf], bias=bias)
                op_dma(C, "sp", dst["gates"][:, tok0:tok0 + 512], og.ap[0:M, :], [og.buf], (), og.chan)
            elif kind == "rope":
                qf = qfring.next()
                op_act(C, qf.ap, bk.ap, AF.Identity, bufs(bk, bfm), [qf.buf], bias=bias)
                rb = rring.next()
                op_mm(C, rb.ap, K.rmat, qf.ap, True, True, [qf.buf, K.t.buf], [rb.buf])
                t1 = t1ring.next()
                t2 = t2ring.next()
                op_tt(C, "pool", t1.ap, qf.ap, K.rope.ap[:, tok0:tok0 + 512], ALU.mult, bufs(qf, K.rope), [t1.buf])
                op_tt(C, "dve", t2.ap, rb.ap, K.rope.ap[:, T + tok0:T + tok0 + 512], ALU.mult, bufs(rb, K.rope), [t2.buf])
                ot = oring.next()
                op_tt(C, "dve", ot.ap, t1.ap, t2.ap, ALU.add, bufs(t1, t2), [ot.buf])
                op_dma(C, "sp", dst["fm"](ci, tt), dst["fm_src"](ci, ot.ap), [ot.buf], (), ot.chan)
            else:
                ot = oring.next()
                func = AF.Sigmoid if kind == "sig" else AF.Identity
                op_act(C, ot.ap, bk.ap, func, bufs(bk, bfm), [ot.buf], bias=bias)
                op_dma(C, "sp", dst["fm"](ci, tt), dst["fm_src"](ci, ot.ap), [ot.buf], (), ot.chan)
        tmc0 = lay["tm_col0"]
        for gi, (g0, n) in enumerate(lay["tm_groups"]):
            ot = tring[gi].next()
            for blk in range(4):
                bk = bring.next()
                for kc in range(16):
                    op_mm(C, bk.ap[:, 0:n], xt.ap[:, kc, blk * 128:(blk + 1) * 128], W.ap[:, kc, tmc0 + g0:tmc0 + g0 + n],
                          kc == 0, False, bufs(W, xt), [bk.buf])
                op_mm(C, bk.ap[:, 0:n], ones_row, btm.ap[0:1, g0:g0 + n], False, True, bufs(K.ones_bf, btm), [bk.buf])
                if blk % 2 == 0:
                    op_copy(C, "dve", ot.ap[:, blk, :], bk.ap[:, 0:n], [bk.buf], [ot.buf])
                else:
                    op_copy(C, "act", ot.ap[:, blk, :], bk.ap[:, 0:n], [bk.buf], [ot.buf])
            op_dma(C, "sp", dst["tm"](gi, tt), ot.ap, [ot.buf], (), ot.chan)


def rows_to_cols(C, K, row_ap, row_buf, nrow, bank, name):
    for b in range(32):
        C.P.op("pe", lambda e, b=b: e.transpose(bank.ap[:, 4 * b:4 * b + nrow], row_ap[0:nrow, b * 128:(b + 1) * 128],
                                                K.ident[0:nrow, 0:nrow]),
               [row_buf, K.t.buf], [bank.buf])
    col = C.tile([128, 32, 4], F32, name)
    op_copy(C, "dve", col.ap.rearrange("p a b -> p (a b)"), bank.ap[:, 0:128], [bank.buf], [col.buf])
    return col


def bcast_tile_ends(C, K, col, bank, name, scale=-1.0, add=0.0):
    rhs = col.ap[:, 3::4, :]
    C.P.op("pe", lambda e: e.matmul(bank.ap[:, 0:32], lhsT=K.e127, rhs=rhs, start=True, stop=True),
           [col.buf, K.t.buf], [bank.buf])
    out = C.tile([128, 8, 4], F32, name)
    op_ts(C, "dve", out.ap.rearrange("p a b -> p (a b)"), bank.ap[:, 0:32], scale, add, ALU.mult, ALU.add, [bank.buf], [out.buf])
    return out


def softplus_neg_rows(C, g, nrow, name):
    op_act(C, g.ap, g.ap, AF.Exp, [g.buf], [g.buf], scale=-1.0)
    op_act(C, g.ap, g.ap, AF.Ln, [g.buf], [g.buf], bias=1.0)
    return g


def cumsum_rows(C, x, zeros, nrow, name):
    out = C.tile([nrow, T], F32, name)
    C.P.op("dve", lambda e: e.tensor_tensor_scan(out=out.ap, data0=x.ap, data1=zeros.ap[0:nrow, :], initial=0.0,
                                                 op0=ALU.add, op1=ALU.add),
           bufs(x, zeros), [out.buf])
    return out


def phase_A2_even(C, K, scr, sinks_dram, ho_send):
    P = C.P
    C.reset()
    g = C.tile([4, T], F32, "g", chan=True)
    op_dma(C, "sp", g.ap, scr["gates"], (), [g.buf], g.chan)
    zeros = C.tile([4, T], F32, "zeros")
    op_memset(C, "pool", zeros.ap, 0.0, [zeros.buf])
    sp = softplus_neg_rows(C, g, 4, "fx")
    Cp = cumsum_rows(C, sp, zeros, 4, "Cp")
    Ccol = rows_to_cols(C, K, Cp.ap, Cp.buf, 4, C.banks[7], "Ccol")
    negCref = bcast_tile_ends(C, K, Ccol, C.banks[6], "negCref", -1.0, 0.0)
    qring = C.ring(2, [128, T], BF16, "fq", chan=True)
    kring = C.ring(2, [128, T], BF16, "fk", chan=True)
    vring = C.ring(2, [128, 32, 128], BF16, "fv", chan=True)
    pring = C.ring(3, [128, 512], BF16, "pT")
    biasring = C.ring(2, [128, 32], F32, "bias")
    rring = C.ring(2, [128, 512], F32, "recip")
    oring = C.ring(2, [128, 512], BF16, "ho", chan=True)
    sring = Ring(C.banks[0:2])
    nring = Ring([C.banks[2], C.banks[4]])
    dring = Ring([C.banks[3], C.banks[5]])
    ones = K.ones_bf

    def load_head(h):
        q, k, v = qring.next(), kring.next(), vring.next()
        op_dma(C, "sp", q.ap, scr["fq"][h], (), [q.buf], q.chan)
        op_dma(C, "sp", k.ap, scr["fk"][h], (), [k.buf], k.chan)
        op_dma(C, "sp", v.ap, scr["fv"][:, h * 128:(h + 1) * 128].rearrange("(b p) d -> p b d", p=128), (), [v.buf], v.chan)
        return q, k, v

    nxt = load_head(0)
    for h in range(4):
        q, k, v = nxt
        if h + 1 < 4:
            nxt = load_head(h + 1)
        for I in range(8):
            nkb = 4 * I + 4
            bias = biasring.next()
            op_ts(C, "dve", bias.ap[:, 0:nkb], Ccol.ap[:, 0:nkb, h], negCref.ap[:, I, h:h + 1], None, ALU.add, None,
                  bufs(Ccol, negCref), [bias.buf])
            nb, db = nring.next(), dring.next()
            for kb in range(nkb):
                j = kb - 4 * I
                off = 128 * j if j > 0 else 0
                sb = sring.next()
                op_mm(C, sb.ap[:, off:512], k.ap[:, kb * 128:(kb + 1) * 128], q.ap[:, I * 512 + off:(I + 1) * 512], True, True,
                      bufs(q, k), [sb.buf])
                pt = pring.next()
                op_act(C, pt.ap[:, off:512], sb.ap[:, off:512], AF.Exp, bufs(sb, bias), [pt.buf],
                       bias=bias.ap[:, kb:kb + 1], scale=SCALE)
                if j >= 0:
                    sub = pt.ap[:, off:off + 128]
                    P.op("pool", lambda e, sub=sub: e.affine_select(out=sub, in_=sub, pattern=[[1, 128]], compare_op=ALU.is_ge,
                                                                    fill=0.0, base=0, channel_multiplier=-1),
                         [pt.buf], [pt.buf])
                op_mm(C, nb.ap[:, off:512], v.ap[:, kb, :], pt.ap[:, off:512], kb == 0, kb == nkb - 1, bufs(v, pt), [nb.buf])
                op_mm(C, db.ap[:, off:512], ones.ap, pt.ap[:, off:512], kb == 0, kb == nkb - 1, bufs(ones, pt), [db.buf])
            rc = rring.next()
            P.op("dve", lambda e, rc=rc, db=db: e.reciprocal(out=rc.ap, in_=db.ap), [db.buf], [rc.buf])
            ot = oring.next()
            op_tt(C, "dve", ot.ap, nb.ap, rc.ap, ALU.mult, bufs(nb, rc), [ot.buf])
            store_ho(C, ho_send, h * 128, I * 512, ot)
    C.reset()
    qs = C.tile([128, 32, 4, 128], BF16, "sq", chan=True)
    op_dma(C, "sp", qs.ap.rearrange("p a b c -> p (a b c)"), scr["sq"].rearrange("p a b c -> p (a b c)"), (), [qs.buf], qs.chan)
    ks = C.tile([128, T], BF16, "sk", chan=True)
    op_dma(C, "sp", ks.ap, scr["sk"], (), [ks.buf], ks.chan)
    vs = C.tile([128, 32, 128], BF16, "sv", chan=True)
    op_dma(C, "sp", vs.ap, scr["sv"].rearrange("(b p) d -> p b d", p=128), (), [vs.buf], vs.chan)
    snk = C.tile([128, 4], F32, "snk", chan=True)
    op_dma(C, "sp", snk.ap, sinks_dram, (), [snk.buf], snk.chan)
    esnk = C.tile([128, 4], F32, "esnk")
    op_act(C, esnk.ap, snk.ap, AF.Exp, [snk.buf], [esnk.buf])
    pcr = C.ring(2, [128, 4, 128], BF16, "pc")
    ppr = C.ring(2, [128, 4, 128], BF16, "pp")
    dnr = C.ring(2, [128, 4, 128], F32, "dn")
    rcr = C.ring(2, [128, 4, 128], F32, "rcs")
    osr = C.ring(2, [128, 4, 128], BF16, "hos", chan=True)
    scr_ = Ring([C.banks[0], C.banks[1]])
    spr_ = Ring([C.banks[2], C.banks[3]])
    nr_ = Ring([C.banks[4], C.banks[6]])
    dr_ = Ring([C.banks[5], C.banks[7]])
    for n in range(32):
        rhs = qs.ap[:, n, :, :].rearrange("p a b -> p (a b)")
        sc_ = scr_.next()
        op_mm(C, sc_.ap, ks.ap[:, n * 128:(n + 1) * 128], rhs, True, True, bufs(ks, qs), [sc_.buf])
        pc = pcr.next()
        op_act(C, pc.ap.rearrange("p a b -> p (a b)"), sc_.ap, AF.Exp, [sc_.buf], [pc.buf], scale=SCALE)
        P.op("pool", lambda e, pc=pc: e.affine_select(out=pc.ap, in_=pc.ap, pattern=[[0, 4], [1, 128]], compare_op=ALU.is_ge,
                                                      fill=0.0, base=0, channel_multiplier=-1), [pc.buf], [pc.buf])
        nb, db = nr_.next(), dr_.next()
        if n > 0:
            sp_ = spr_.next()
            op_mm(C, sp_.ap, ks.ap[:, (n - 1) * 128:n * 128], rhs, True, True, bufs(ks, qs), [sp_.buf])
            pp = ppr.next()
            op_act(C, pp.ap.rearrange("p a b -> p (a b)"), sp_.ap, AF.Exp, [sp_.buf], [pp.buf], scale=SCALE)
            P.op("pool", lambda e, pp=pp: e.affine_select(out=pp.ap, in_=pp.ap, pattern=[[0, 4], [-1, 128]], compare_op=ALU.is_ge,
                                                          fill=0.0, base=-1, channel_multiplier=1), [pp.buf], [pp.buf])
        pcf = pc.ap.rearrange("p a b -> p (a b)")
        op_mm(C, nb.ap, vs.ap[:, n, :], pcf, True, n == 0, bufs(vs, pc), [nb.buf])
        op_mm(C, db.ap, ones.ap, pcf, True, n == 0, bufs(ones, pc), [db.buf])
        if n > 0:
            ppf = pp.ap.rearrange("p a b -> p (a b)")
            op_mm(C, nb.ap, vs.ap[:, n - 1, :], ppf, False, True, bufs(vs, pp), [nb.buf])
            op_mm(C, db.ap, ones.ap, ppf, False, True, bufs(ones, pp), [db.buf])
        dn = dnr.next()
        op_tt(C, "dve", dn.ap, db.ap.rearrange("p (a b) -> p a b", a=4), esnk.ap[:, :, None].to_broadcast([128, 4, 128]), ALU.add,
              bufs(db, esnk), [dn.buf])
        rc = rcr.next()
        P.op("dve", lambda e, rc=rc, dn=dn: e.reciprocal(out=rc.ap, in_=dn.ap), [dn.buf], [rc.buf])
        ot = osr.next()
        op_tt(C, "dve", ot.ap, nb.ap.rearrange("p (a b) -> p a b", a=4), rc.ap, ALU.mult, bufs(nb, rc), [ot.buf])
        for hq in range(4):
            store_ho(C, ho_send, 512 + hq * 128, n * 128, ot, src_ap=ot.ap[:, hq, :], ntok=128)


def store_ho(C, ho_send, f0, t0, ot, src_ap=None, ntok=512):
    if src_ap is None:
        src_ap = ot.ap
    j = t0 // TH
    c0 = t0 - (TH * j - 2)
    op_dma(C, "sp", ho_send[j, f0:f0 + 128, c0:c0 + ntok], src_ap, [ot.buf], (), ot.chan)
    if j == 0 and t0 + ntok == TH:
        op_dma(C, "sp", ho_send[1, f0:f0 + 128, 0:2], src_ap[:, ntok - 2:ntok], [ot.buf], (), ot.chan)


def new_nc():
    return bass.Bass("TRN2", target_bir_lowering=False)


def dram(nc, name, shape, dt, kind):
    return nc.dram_tensor(name, list(shape), dt, kind=kind).ap()


EVEN_LAY = dict(ncols=2308, nfm=14, gate_m=4, ntm=640, tm_col0=1668, tm_groups=[(0, 512), (512, 128)],
                kinds=["plain"] * 8 + ["rope"] * 5 + ["gate"])


def zero_halo(C, ho_send):
    z = C.tile([128, 8, 2], BF16, "zh", chan=True)
    op_memset(C, "pool", z.ap, 0.0, [z.buf])
    op_dma(C, "sp", ho_send[0, :, 0:2].rearrange("(c p) t -> p c t", p=128), z.ap, [z.buf], (), z.chan)
    return z


def emit_A_even(C, K, xsrc, x_f32, w, bfm, btm, sinks, scr, ho_send):
    def fm_dst(ci, tt):
        t0 = tt * 512
        if ci < 4:
            return scr["fq"][ci][:, t0:t0 + 512]
        if ci < 8:
            return scr["fk"][ci - 4][:, t0:t0 + 512]
        if ci < 12:
            return scr["sq"][:, 4 * tt:4 * tt + 4, ci - 8, :]
        return scr["sk"][:, t0:t0 + 512]

    def fm_src(ci, ap):
        if 8 <= ci < 12:
            return ap.rearrange("p (a b) -> p a b", a=4)
        return ap

    def tm_dst(gi, tt):
        t0 = tt * 512
        d = scr["fv"] if gi == 0 else scr["sv"]
        return d[t0:t0 + 512, :].rearrange("(b p) c -> p b c", p=128)

    dst = dict(gates=scr["gates"], fm=fm_dst, fm_src=fm_src, tm=tm_dst)
    phase_A1(C, K, EVEN_LAY, xsrc, x_f32, w, bfm, btm, dst)
    phase_A2_even(C, K, scr, sinks, ho_send)


def even_scratch(nc, kind="Internal"):
    return dict(
        fq=[dram(nc, "s_fq%d" % h, [128, T], BF16, kind) for h in range(4)],
        fk=[dram(nc, "s_fk%d" % h, [128, T], BF16, kind) for h in range(4)],
        sq=dram(nc, "s_sq", [128, 32, 4, 128], BF16, kind),
        sk=dram(nc, "s_sk", [128, T], BF16, kind),
        gates=dram(nc, "s_gates", [4, T], F32, kind),
        fv=dram(nc, "s_fv", [T, 512], BF16, kind),
        sv=dram(nc, "s_sv", [T, 128], BF16, kind),
    )


def build_A_even(x_f32):
    nc = new_nc()
    xdt = F32 if x_f32 else BF16
    xsrc = dram(nc, "xsrc", [2, D, TH], xdt, "ExternalInput")
    w = dram(nc, "w", [128, 16, 2308], F32, "ExternalInput")
    bfm = dram(nc, "bfm", [128, 14], F32, "ExternalInput")
    btm = dram(nc, "btm", [1, 640], F32, "ExternalInput")
    sinks = dram(nc, "sinks", [128, 4], F32, "ExternalInput")
    cst = dram(nc, "cst", [128, CST_COLS], F32, "ExternalInput")
    rope = dram(nc, "rope", [128, 2 * T], F32, "ExternalInput")
    ho = dram(nc, "ho", [2, 1024, NH], BF16, "ExternalOutput")
    scr = even_scratch(nc)
    with contextlib.ExitStack() as st:
        C = Ctx(nc, st)
        K = load_consts(C, cst, True, rope)
        zh = zero_halo(C, ho)
        C.persist = C.off
        emit_A_even(C, K, xsrc, x_f32, w, bfm, btm, sinks, scr, ho)
        C.P.emit(final_chans=[c for c in C.P.chans])
    return nc


def wlayout(wc):
    return np.ascontiguousarray(wc.reshape(16, 128, -1).transpose(1, 0, 2))


def even_cols(hh):
    r = np.arange
    fq = 512 * hh + r(512)
    fk = 1024 + 512 * hh + r(512)
    sq = 3080 + 512 * hh + r(512)
    sk = 4104 + 128 * hh + r(128)
    ff = 3072 + 4 * hh + r(4)
    fv = 2048 + 512 * hh + r(512)
    sv = 4360 + 128 * hh + r(128)
    return np.concatenate([fq, fk, sq, sk]), ff, np.concatenate([fv, sv])


def even_inputs(w_in, b_in, sinks, hh):
    fm, gate, tm = even_cols(hh)
    cols = np.concatenate([fm, gate, tm])
    w = wlayout(w_in[:, cols])
    bfm = np.zeros((128, 14), np.float32)
    bfm[:, 0:13] = b_in[fm].reshape(13, 128).T
    bfm[0:4, 13] = b_in[gate]
    btm = np.ascontiguousarray(b_in[tm][None, :])
    sk = np.ascontiguousarray(np.broadcast_to(sinks[4 * hh:4 * hh + 4][None, :], (128, 4)))
    return dict(w=w, bfm=bfm, btm=btm, sinks=sk)


ODD_LAY = dict(ncols=3080, nfm=17, gate_m=8, ntm=1024, tm_col0=2056, tm_groups=[(0, 512), (512, 512)],
               kinds=["plain"] * 8 + ["sig"] * 8 + ["gate"])
LN_SCALE = float(np.log(128 ** -0.5))


def phase_A2_odd(C, K, scr, ho_send):
    P = C.P
    C.reset()
    gi = C.tile([4, T], F32, "gi", chan=True)
    op_dma(C, "sp", gi.ap, scr["gates"][0:4, :], (), [gi.buf], gi.chan)
    gf = C.tile([4, T], F32, "gf", chan=True)
    op_dma(C, "sp", gf.ap, scr["gates"][4:8, :], (), [gf.buf], gf.chan)
    zeros = C.tile([4, T], F32, "zeros")
    op_memset(C, "pool", zeros.ap, 0.0, [zeros.buf])
    sp = softplus_neg_rows(C, gf, 4, "ml")
    Fm = cumsum_rows(C, sp, zeros, 4, "Fm")
    a = gi
    op_tt(C, "dve", a.ap, gi.ap, Fm.ap, ALU.add, bufs(gi, Fm), [a.buf])
    M = C.tile([4, T], F32, "M")
    P.op("dve", lambda e: e.tensor_tensor_scan(out=M.ap, data0=a.ap, data1=a.ap, initial=-1e30, op0=ALU.max, op1=ALU.max),
         [a.buf], [M.buf])
    acol = rows_to_cols(C, K, a.ap, a.buf, 4, C.banks[7], "acol")
    Mcol = rows_to_cols(C, K, M.ap, M.buf, 4, C.banks[6], "Mcol")
    negG = bcast_tile_ends(C, K, Mcol, C.banks[5], "negG", -1.0, LN_SCALE)
    negGrow = C.tile([4, 8], F32, "negGrow")
    op_ts(C, "dve", negGrow.ap, M.ap[:, 511::512], -1.0, None, ALU.mult, None, [M.buf], [negGrow.buf])
    rowe = gf
    for I in range(8):
        op_act(C, rowe.ap[:, I * 512:(I + 1) * 512], Fm.ap[:, I * 512:(I + 1) * 512], AF.Exp, bufs(Fm, negGrow), [rowe.buf],
               bias=negGrow.ap[:, I:I + 1])
    qring = C.ring(2, [128, T], BF16, "mq", chan=True)
    kring = C.ring(2, [128, T], BF16, "mk", chan=True)
    vring = C.ring(2, [128, 32, 256], BF16, "mv", chan=True)
    pring = C.ring(3, [128, 512], BF16, "pT")
    wring = C.ring(2, [128, 32], F32, "wcol")
    ering = C.ring(2, [128, 512], F32, "esb")
    dring = C.ring(2, [128, 512], F32, "den")
    rring = C.ring(2, [128, 512], F32, "recip")
    hring = C.ring(2, [128, 512], F32, "hh")
    sgring = C.ring(3, [128, 512], BF16, "sg", chan=True)
    oring = C.ring(3, [128, 512], BF16, "ho", chan=True)
    sring = Ring(C.banks[0:2])
    accs = Ring([(C.banks[2], C.banks[3], C.banks[4]), (C.banks[5], C.banks[6], C.banks[7])])
    ones = K.ones_bf

    def load_head(h):
        q, k, v = qring.next(), kring.next(), vring.next()
        op_dma(C, "sp", q.ap, scr["q"][h], (), [q.buf], q.chan)
        op_dma(C, "sp", k.ap, scr["k"][h], (), [k.buf], k.chan)
        op_dma(C, "sp", v.ap, scr["v"][:, h * 256:(h + 1) * 256].rearrange("(b p) d -> p b d", p=128), (), [v.buf], v.chan)
        return q, k, v

    nxt = load_head(0)
    cnt = 0
    for h in range(4):
        q, k, v = nxt
        if h + 1 < 4:
            nxt = load_head(h + 1)
        for I in range(8):
            nkb = 4 * I + 4
            wc = wring.next()
            op_act(C, wc.ap[:, 0:nkb], acol.ap[:, 0:nkb, h], AF.Exp, bufs(acol, negG), [wc.buf], bias=negG.ap[:, I, h:h + 1])
            sg = []
            for c in range(2):
                s_ = sgring.next()
                op_dma(C, "sp", s_.ap, scr["so"][2 * h + c][:, I * 512:(I + 1) * 512], (), [s_.buf], s_.chan)
                sg.append(s_)
            n0, n1, dt = accs.next()
            for kb in range(nkb):
                j = kb - 4 * I
                off = 128 * j if j > 0 else 0
                sb = sring.next()
                op_mm(C, sb.ap[:, off:512], k.ap[:, kb * 128:(kb + 1) * 128], q.ap[:, I * 512 + off:(I + 1) * 512], True, True,
                      bufs(q, k), [sb.buf])
                pt = pring.next()
                cnt += 1
                if cnt % 2 == 0:
                    op_act(C, pt.ap[:, off:512], sb.ap[:, off:512], AF.Copy, bufs(sb, wc), [pt.buf], scale=wc.ap[:, kb:kb + 1])
                else:
                    op_ts(C, "dve", pt.ap[:, off:512], sb.ap[:, off:512], wc.ap[:, kb:kb + 1], None, ALU.mult, None,
                          bufs(sb, wc), [pt.buf])
                if j >= 0:
                    sub = pt.ap[:, off:off + 128]
                    P.op("pool", lambda e, sub=sub: e.affine_select(out=sub, in_=sub, pattern=[[1, 128]], compare_op=ALU.is_ge,
                                                                    fill=0.0, base=0, channel_multiplier=-1),
                         [pt.buf], [pt.buf])
                last = kb == nkb - 1
                op_mm(C, n0.ap[:, off:512], v.ap[:, kb, 0:128], pt.ap[:, off:512], kb == 0, last, bufs(v, pt), [n0.buf])
                op_mm(C, n1.ap[:, off:512], v.ap[:, kb, 128:256], pt.ap[:, off:512], kb == 0, last, bufs(v, pt), [n1.buf])
                op_mm(C, dt.ap[:, off:512], ones.ap, pt.ap[:, off:512], kb == 0, last, bufs(ones, pt), [dt.buf])
            eb = sring.next()
            op_mm(C, eb.ap, K.sel[h], rowe.ap[0:4, I * 512:(I + 1) * 512], True, True, [rowe.buf, K.t.buf], [eb.buf])
            es = ering.next()
            op_copy(C, "act", es.ap, eb.ap, [eb.buf], [es.buf])
            dn = dring.next()
            op_act(C, dn.ap, dt.ap, AF.Abs, [dt.buf], [dn.buf])
            op_tt(C, "dve", dn.ap, dn.ap, es.ap, ALU.max, bufs(dn, es), [dn.buf])
            rc = rring.next()
            P.op("dve", lambda e, rc=rc, dn=dn: e.reciprocal(out=rc.ap, in_=dn.ap), [dn.buf], [rc.buf])
            for c, nb in enumerate((n0, n1)):
                hh_ = hring.next()
                op_tt(C, "dve", hh_.ap, nb.ap, rc.ap, ALU.mult, bufs(nb, rc), [hh_.buf])
                ot = oring.next()
                op_tt(C, "pool", ot.ap, hh_.ap, sg[c].ap, ALU.mult, bufs(hh_, sg[c]), [ot.buf])
                store_ho(C, ho_send, h * 256 + c * 128, I * 512, ot)


def emit_A_odd(C, K, xsrc, w, bfm, btm, scr, ho_send):
    def fm_dst(ci, tt):
        t0 = tt * 512
        if ci < 4:
            return scr["q"][ci][:, t0:t0 + 512]
        if ci < 8:
            return scr["k"][ci - 4][:, t0:t0 + 512]
        return scr["so"][ci - 8][:, t0:t0 + 512]

    def tm_dst(gi, tt):
        t0 = tt * 512
        return scr["v"][t0:t0 + 512, gi * 512:(gi + 1) * 512].rearrange("(b p) c -> p b c", p=128)

    dst = dict(gates=scr["gates"], fm=fm_dst, fm_src=lambda ci, ap: ap, tm=tm_dst)
    phase_A1(C, K, ODD_LAY, xsrc, False, w, bfm, btm, dst)
    phase_A2_odd(C, K, scr, ho_send)


def odd_scratch(nc, kind="Internal"):
    return dict(
        q=[dram(nc, "s_q%d" % h, [128, T], BF16, kind) for h in range(4)],
        k=[dram(nc, "s_k%d" % h, [128, T], BF16, kind) for h in range(4)],
        so=[dram(nc, "s_so%d" % h, [128, T], BF16, kind) for h in range(8)],
        gates=dram(nc, "s_gates", [8, T], F32, kind),
        v=dram(nc, "s_v", [T, 1024], BF16, kind),
    )


def build_A_odd():
    nc = new_nc()
    xsrc = dram(nc, "xsrc", [2, D, TH], BF16, "ExternalInput")
    w = dram(nc, "w", [128, 16, 3080], F32, "ExternalInput")
    bfm = dram(nc, "bfm", [128, 17], F32, "ExternalInput")
    btm = dram(nc, "btm", [1, 1024], F32, "ExternalInput")
    cst = dram(nc, "cst", [128, CST_COLS], F32, "ExternalInput")
    ho = dram(nc, "ho", [2, 1024, NH], BF16, "ExternalOutput")
    scr = odd_scratch(nc)
    with contextlib.ExitStack() as st:
        C = Ctx(nc, st)
        K = load_consts(C, cst, False)
        zh = zero_halo(C, ho)
        C.persist = C.off
        emit_A_odd(C, K, xsrc, w, bfm, btm, scr, ho)
        C.P.emit(final_chans=[c for c in C.P.chans])
    return nc


def odd_cols(hh):
    r = np.arange
    q = 512 * hh + r(512)
    k = 1024 + 512 * hh + r(512)
    o = 4096 + 1024 * hh + r(1024)
    ig = 6144 + 4 * hh + r(4)
    fg = 6152 + 4 * hh + r(4)
    v = 2048 + 1024 * hh + r(1024)
    return np.concatenate([q, k, o]), np.concatenate([ig, fg]), v


def odd_inputs(w_in, b_in, hh):
    fm, gate, tm = odd_cols(hh)
    cols = np.concatenate([fm, gate, tm])
    w = wlayout(w_in[:, cols])
    bfm = np.zeros((128, 17), np.float32)
    bfm[:, 0:16] = b_in[fm].reshape(16, 128).T
    bfm[0:8, 16] = b_in[gate]
    btm = np.ascontiguousarray(b_in[tm][None, :])
    return dict(w=w, bfm=bfm, btm=btm)


class LNRes:
    pass


def ln_alloc(C):
    R = LNRes()
    R.zb = C.ring(2, [128, 512], BF16, "zb")
    R.zq = C.ring(2, [128, 512], BF16, "zq")
    R.mean = C.tile([128, 512], F32, "mean")
    R.var = C.tile([128, 512], F32, "var")
    R.rstd = C.tile([128, 512], F32, "rstd")
    R.t = C.ring(2, [128, 512], F32, "lnt")
    return R


def ln_fm(C, K, R, z, n, gcol, bcol, s1, s2, out_bf):
    ones = K.ones_bf
    for c in range(16):
        zb = R.zb.next()
        op_copy(C, "act", zb.ap[:, 0:n], z.ap[:, c, 0:n], [z.buf], [zb.buf])
        zq = R.zq.next()
        op_act(C, zq.ap[:, 0:n], z.ap[:, c, 0:n], AF.Square, [z.buf], [zq.buf])
        op_mm(C, s1.ap[:, 0:n], ones.ap, zb.ap[:, 0:n], c == 0, c == 15, bufs(ones, zb), [s1.buf])
        op_mm(C, s2.ap[:, 0:n], ones.ap, zq.ap[:, 0:n], c == 0, c == 15, bufs(ones, zq), [s2.buf])
    m, v, r = R.mean, R.var, R.rstd
    C.P.op("act", lambda e: e.mul(out=m.ap[:, 0:n], in_=s1.ap[:, 0:n], mul=1.0 / D), [s1.buf], [m.buf])
    op_tt(C, "dve", v.ap[:, 0:n], m.ap[:, 0:n], m.ap[:, 0:n], ALU.mult, [m.buf], [v.buf])
    op_stt(C, v.ap[:, 0:n], s2.ap[:, 0:n], 1.0 / D, v.ap[:, 0:n], ALU.mult, ALU.subtract, bufs(s2, v), [v.buf])
    op_ts(C, "dve", v.ap[:, 0:n], v.ap[:, 0:n], EPS, None, ALU.add, None, [v.buf], [v.buf])
    C.P.op("act", lambda e: e.sqrt(out=r.ap[:, 0:n], in_=v.ap[:, 0:n]), [v.buf], [r.buf])
    C.P.op("dve", lambda e: e.reciprocal(out=r.ap[:, 0:n], in_=r.ap[:, 0:n]), [r.buf], [r.buf])
    for c in range(16):
        t = R.t.next()
        op_tt(C, "dve", t.ap[:, 0:n], z.ap[:, c, 0:n], m.ap[:, 0:n], ALU.subtract, bufs(z, m), [t.buf])
        op_tt(C, "pool", t.ap[:, 0:n], t.ap[:, 0:n], r.ap[:, 0:n], ALU.mult, bufs(t, r), [t.buf])
        op_act(C, z.ap[:, c, 0:n], t.ap[:, 0:n], AF.Identity, bufs(t, gcol, bcol), [z.buf],
               bias=bcol.ap[:, c:c + 1], scale=gcol.ap[:, c:c + 1])
        dst_ap, dst_t = out_bf(c)
        op_copy(C, "pool", dst_ap, z.ap[:, c, 0:n], [z.buf], [dst_t.buf])


B_TILES = [(0, 2)] + [(2 + 512 * i, 512) for i in range(4)]


def phase_B1(C, K, ho_recv, w_out, xres, ln1, flag_dram, x1T, x1nT):
    P = C.P
    C.reset()
    Wo = C.tile([128, 16, D], BF16, "Wo", chan=True)
    op_dma(C, "pool", Wo.ap.rearrange("p a b -> p (a b)"), w_out.rearrange("p a b -> p (a b)"), (), [Wo.buf], Wo.chan,
           max_dma_last_dim=4096)
    g1 = C.tile([128, 16], F32, "g1", chan=True)
    op_dma(C, "sp", g1.ap, ln1[0], (), [g1.buf], g1.chan)
    b1 = C.tile([128, 16], F32, "b1", chan=True)
    op_dma(C, "sp", b1.ap, ln1[1], (), [b1.buf], b1.chan)
    flag = C.tile([128, 1], F32, "flag", chan=True)
    op_dma(C, "sp", flag.ap, flag_dram, (), [flag.buf], flag.chan)
    R = ln_alloc(C)
    horing = C.ring(2, [128, 16, 512], BF16, "hoT", chan=True)
    zring = C.ring(2, [128, 16, 512], F32, "z", chan=True)
    obring = C.ring(1, [128, 16, 512], BF16, "x1n", chan=True)
    bring = Ring(C.banks[0:6])

    def load(ti):
        c0, n = B_TILES[ti]
        ho = horing.next()
        op_dma(C, "sp", ho.ap[:, :, 0:n], ho_recv[:, :, c0:c0 + n].rearrange("r (c p) t -> p (r c) t", p=128), (), [ho.buf], ho.chan)
        z = zring.next()
        op_dma(C, "sp", z.ap[:, :, 0:n], xres[:, c0:c0 + n].rearrange("(c p) t -> p c t", p=128), (), [z.buf], z.chan)
        return ho, z

    nxt = load(0)
    for ti, (c0, n) in enumerate(B_TILES):
        ho, z = nxt
        if ti + 1 < len(B_TILES):
            nxt = load(ti + 1)
        for c in range(16):
            bk = bring.next()
            for kc in range(16):
                op_mm(C, bk.ap[:, 0:n], Wo.ap[:, kc, c * 128:(c + 1) * 128], ho.ap[:, kc, 0:n], kc == 0, kc == 15,
                      bufs(Wo, ho), [bk.buf])
            op_stt(C, z.ap[:, c, 0:n], z.ap[:, c, 0:n], ALPHA, bk.ap[:, 0:n], ALU.mult, ALU.add, bufs(z, bk), [z.buf])
        ob = obring.next()
        ln_fm(C, K, R, z, n, g1, b1, C.banks[6], C.banks[7], lambda c, ob=ob, n=n: (ob.ap[:, c, 0:n], ob))
        if ti == 0:
            op_ts(C, "dve", ob.ap[:, :, 0:n], ob.ap[:, :, 0:n], flag.ap[:, 0:1], None, ALU.mult, None, bufs(ob, flag), [ob.buf])
        op_dma(C, "sp", x1T[:, c0:c0 + n].rearrange("(c p) t -> p c t", p=128), z.ap[:, :, 0:n], [z.buf], (), z.chan)
        op_dma(C, "sp", x1nT[:, c0:c0 + n].rearrange("(c p) t -> p c t", p=128), ob.ap[:, :, 0:n], [ob.buf], (), ob.chan)


def phase_B2(C, K, x1nT, w_up, convp, actT):
    P = C.P
    C.reset()
    X = C.tile([128, 16, NH], BF16, "X1T", chan=True)
    op_dma(C, "sp", X.ap, x1nT.rearrange("(c p) t -> p c t", p=128), (), [X.buf], X.chan)
    cp = C.tile([128, NJ, 8], F32, "convp", chan=True)
    op_dma(C, "sp", cp.ap, convp, (), [cp.buf], cp.chan)
    slab = C.ring(3, [128, 16, 256], BF16, "slab", chan=True)
    U = [[C.tile([128, 514], F32, "U%d%d" % (s, gv)) for gv in range(2)] for s in range(2)]
    t1r = C.ring(2, [128, 512], F32, "t1")
    t2r = C.ring(2, [128, 512], F32, "t2")
    cgr = C.ring(2, [128, 512], F32, "cg")
    cvr = C.ring(2, [128, 512], F32, "cv")
    sgr = C.ring(2, [128, 512], F32, "sg")
    aor = C.ring(3, [128, 512], BF16, "ao", chan=True)
    bring = Ring(C.banks)

    def load_slab(j):
        s = slab.next()
        op_dma(C, "pool", s.ap.rearrange("p a b -> p (a b)"), w_up[j].rearrange("p a b -> p (a b)"), (), [s.buf], s.chan,
               max_dma_last_dim=4096)
        return s

    pend = [load_slab(0), load_slab(1)]
    si = 0
    for j in range(NJ):
        s = pend.pop(0)
        if j + 2 < NJ:
            pend.append(load_slab(j + 2))
        for ti, (c0, n) in enumerate(B_TILES):
            bg, bv = bring.next(), bring.next()
            for gv, bk in enumerate((bg, bv)):
                for kc in range(16):
                    op_mm(C, bk.ap[:, 0:n], s.ap[:, kc, gv * 128:(gv + 1) * 128], X.ap[:, kc, c0:c0 + n], kc == 0, kc == 15,
                          bufs(s, X), [bk.buf])
            if ti == 0:
                for gv, bk in enumerate((bg, bv)):
                    op_copy(C, "act", U[si][gv].ap[:, 0:2], bk.ap[:, 0:2], [bk.buf], [U[si][gv].buf])
                continue
            Ug, Uv = U[si]
            op_copy(C, "act", Ug.ap[:, 2:514], bg.ap, [bg.buf], [Ug.buf])
            op_copy(C, "act", Uv.ap[:, 2:514], bv.ap, [bv.buf], [Uv.buf])
            outs = []
            for gv, Uu, rr in ((0, Ug, cgr), (1, Uv, cvr)):
                o = 4 * gv
                t1 = t1r.next()
                op_act(C, t1.ap, Uu.ap[:, 0:512], AF.Identity, bufs(Uu, cp), [t1.buf],
                       bias=cp.ap[:, j, o + 3:o + 4], scale=cp.ap[:, j, o:o + 1])
                t2 = t2r.next()
                op_stt(C, t2.ap, Uu.ap[:, 1:513], cp.ap[:, j, o + 1:o + 2], t1.ap, ALU.mult, ALU.add, bufs(Uu, cp, t1), [t2.buf])
                cc = rr.next()
                op_stt(C, cc.ap, Uu.ap[:, 2:514], cp.ap[:, j, o + 2:o + 3], t2.ap, ALU.mult, ALU.add, bufs(Uu, cp, t2), [cc.buf])
                outs.append(cc)
            nsi = 1 - si
            for gv in range(2):
                op_copy(C, "pool", U[nsi][gv].ap[:, 0:2], U[si][gv].ap[:, 512:514], [U[si][gv].buf], [U[nsi][gv].buf])
            si = nsi
            sg = sgr.next()
            op_act(C, sg.ap, outs[0].ap, AF.Silu, [outs[0].buf], [sg.buf])
            ao = aor.next()
            op_tt(C, "pool", ao.ap, sg.ap, outs[1].ap, ALU.mult, bufs(sg, outs[1]), [ao.buf])
            op_dma(C, "sp", actT[j * 128:(j + 1) * 128, c0 - 2:c0 - 2 + 512], ao.ap, [ao.buf], (), ao.chan)


def phase_B3(C, K, actT, w_down, x1T, ln2, xT_out, xn_out):
    P = C.P
    C.reset()
    g2 = C.tile([128, 16], F32, "g2", chan=True)
    op_dma(C, "sp", g2.ap, ln2[0], (), [g2.buf], g2.chan)
    b2 = C.tile([128, 16], F32, "b2", chan=True)
    op_dma(C, "sp", b2.ap, ln2[1], (), [b2.buf], b2.chan)
    R = ln_alloc(C)
    A = C.tile([128, NJ, 1024], BF16, "actT", chan=True)
    Z = C.tile([128, 16, 1024], F32, "Z", chan=True)
    wdr = C.ring(4, [128, 22, 128], BF16, "wd", chan=True)
    x1r = C.ring(2, [128, 512], F32, "x1c", chan=True)
    obr = C.ring(2, [128, 512], BF16, "xnb", chan=True)
    bring = Ring(C.banks[0:6])

    def load_wd(n, half):
        w = wdr.next()
        op_dma(C, "pool", w.ap.rearrange("p a b -> p (a b)"), w_down[n, :, half * 22:(half + 1) * 22, :].rearrange("p a b -> p (a b)"),
               (), [w.buf], w.chan, max_dma_last_dim=4096)
        return w

    for th in range(2):
        op_dma(C, "sp", A.ap, actT[:, th * 1024:(th + 1) * 1024].rearrange("(j p) t -> p j t", p=128), (), [A.buf], A.chan)
        pend = [(load_wd(0, 0), load_wd(0, 1))]
        for n in range(16):
            wa, wb = pend.pop(0)
            if n + 1 < 16:
                pend.append((load_wd(n + 1, 0), load_wd(n + 1, 1)))
            for t2 in range(2):
                bk = bring.next()
                for j in range(NJ):
                    w = wa if j < 22 else wb
                    op_mm(C, bk.ap, w.ap[:, j % 22, :], A.ap[:, j, t2 * 512:(t2 + 1) * 512], j == 0, j == NJ - 1, bufs(w, A), [bk.buf])
                x1c = x1r.next()
                col = 2 + th * 1024 + t2 * 512
                op_dma(C, "sp", x1c.ap, x1T[n * 128:(n + 1) * 128, col:col + 512], (), [x1c.buf], x1c.chan)
                op_stt(C, Z.ap[:, n, t2 * 512:(t2 + 1) * 512], x1c.ap, ALPHA, bk.ap, ALU.mult, ALU.add, bufs(x1c, bk), [Z.buf])
        for t2 in range(2):
            tok = th * 1024 + t2 * 512
            zt = Tile(Z.ap[:, :, t2 * 512:(t2 + 1) * 512], Z.buf, Z.chan)

            def out_bf(c, tok=tok):
                ob = obr.next()
                out_bf.last = ob
                return ob.ap, ob

            ln_fm_store(C, K, R, zt, g2, b2, C.banks[6], C.banks[7], obr, xn_out, tok)
            op_dma(C, "sp", xT_out[:, tok:tok + 512].rearrange("(c p) t -> p c t", p=128), zt.ap, [Z.buf], (), Z.chan)


def ln_fm_store(C, K, R, z, gcol, bcol, s1, s2, obr, xn_out, tok):
    n = 512
    ones = K.ones_bf
    for c in range(16):
        zb = R.zb.next()
        op_copy(C, "act", zb.ap, z.ap[:, c, :], [z.buf], [zb.buf])
        zq = R.zq.next()
        op_act(C, zq.ap, z.ap[:, c, :], AF.Square, [z.buf], [zq.buf])
        op_mm(C, s1.ap, ones.ap, zb.ap, c == 0, c == 15, bufs(ones, zb), [s1.buf])
        op_mm(C, s2.ap, ones.ap, zq.ap, c == 0, c == 15, bufs(ones, zq), [s2.buf])
    m, v, r = R.mean, R.var, R.rstd
    C.P.op("act", lambda e: e.mul(out=m.ap, in_=s1.ap, mul=1.0 / D), [s1.buf], [m.buf])
    op_tt(C, "dve", v.ap, m.ap, m.ap, ALU.mult, [m.buf], [v.buf])
    op_stt(C, v.ap, s2.ap, 1.0 / D, v.ap, ALU.mult, ALU.subtract, bufs(s2, v), [v.buf])
    op_ts(C, "dve", v.ap, v.ap, EPS, None, ALU.add, None, [v.buf], [v.buf])
    C.P.op("act", lambda e: e.sqrt(out=r.ap, in_=v.ap), [v.buf], [r.buf])
    C.P.op("dve", lambda e: e.reciprocal(out=r.ap, in_=r.ap), [r.buf], [r.buf])
    for c in range(16):
        t = R.t.next()
        op_tt(C, "dve", t.ap, z.ap[:, c, :], m.ap, ALU.subtract, bufs(z, m), [t.buf])
        op_tt(C, "pool", t.ap, t.ap, r.ap, ALU.mult, bufs(t, r), [t.buf])
        op_act(C, z.ap[:, c, :], t.ap, AF.Identity, bufs(t, gcol, bcol), [z.buf], bias=bcol.ap[:, c:c + 1], scale=gcol.ap[:, c:c + 1])
        ob = obr.next()
        op_copy(C, "pool", ob.ap, z.ap[:, c, :], [z.buf], [ob.buf])
        op_dma(C, "sp", xn_out[c * 128:(c + 1) * 128, tok:tok + 512], ob.ap, [ob.buf], (), ob.chan)


def build_B():
    nc = new_nc()
    ho_recv = dram(nc, "ho_recv", [2, 1024, NH], BF16, "ExternalInput")
    w_out = dram(nc, "w_out", [128, 16, D], F32, "ExternalInput")
    xres = dram(nc, "xres", [D, NH], F32, "ExternalInput")
    ln1g = dram(nc, "ln1g", [128, 16], F32, "ExternalInput")
    ln1b = dram(nc, "ln1b", [128, 16], F32, "ExternalInput")
    ln2g = dram(nc, "ln2g", [128, 16], F32, "ExternalInput")
    ln2b = dram(nc, "ln2b", [128, 16], F32, "ExternalInput")
    flag = dram(nc, "flag", [128, 1], F32, "ExternalInput")
    w_up = dram(nc, "w_up", [NJ, 128, 16, 256], F32, "ExternalInput")
    convp = dram(nc, "convp", [128, NJ, 8], F32, "ExternalInput")
    w_down = dram(nc, "w_down", [16, 128, NJ, 128], F32, "ExternalInput")
    cst = dram(nc, "cst", [128, CST_COLS], F32, "ExternalInput")
    xT_out = dram(nc, "xT_out", [D, TH], F32, "ExternalOutput")
    xn_out = dram(nc, "xn_out", [D, TH], BF16, "ExternalOutput")
    x1T = dram(nc, "s_x1T", [D, NH], F32, "Internal")
    x1nT = dram(nc, "s_x1nT", [D, NH], BF16, "Internal")
    actT = dram(nc, "s_actT", [DFF, TH], BF16, "Internal")
    with contextlib.ExitStack() as st:
        C = Ctx(nc, st)
        K = load_consts(C, cst, False)
        phase_B1(C, K, ho_recv, w_out, xres, (ln1g, ln1b), flag, x1T, x1nT)
        phase_B2(C, K, x1nT, w_up, convp, actT)
        phase_B3(C, K, actT, w_down, x1T, (ln2g, ln2b), xT_out, xn_out)
        C.P.emit(final_chans=[c for c in C.P.chans])
    return nc


def colvec(v):
    return np.ascontiguousarray(v.reshape(16, 128).T)


def wout_rows(layer_even):
    if layer_even:
        r = np.arange
        return np.concatenate([np.concatenate([512 * rk + r(512), 1024 + 512 * rk + r(512)]) for rk in range(2)])
    return np.arange(2048)


def b_weight_inputs(inp, layer):
    j = layer // 2
    even = layer % 2 == 0
    w_out = (inp["attn_w_out"] if even else inp["mlstm_w_out"])[j]
    w_up = inp["ffn_w_up"][layer]
    wu = np.concatenate([w_up[:, :DFF].reshape(D, NJ, 1, 128), w_up[:, DFF:].reshape(D, NJ, 1, 128)], axis=2)
    wu = np.ascontiguousarray(wu.reshape(16, 128, NJ, 256).transpose(2, 1, 0, 3))
    cw, cb = inp["ffn_conv_w"][layer], inp["ffn_conv_b"][layer]
    convp = np.zeros((128, NJ, 8), np.float32)
    for gv in range(2):
        o = gv * DFF
        for k in range(3):
            convp[:, :, 4 * gv + k] = cw[k, o:o + DFF].reshape(NJ, 128).T
        convp[:, :, 4 * gv + 3] = cb[o:o + DFF].reshape(NJ, 128).T
    wd = inp["ffn_w_down"][layer]
    wd = np.ascontiguousarray(wd.reshape(NJ, 128, 16, 128).transpose(2, 1, 0, 3))
    return dict(w_out=wlayout(w_out[wout_rows(even)]), w_up=wu, convp=convp, w_down=wd,
                ln1g=colvec(inp["ln1_g"][layer]), ln1b=colvec(inp["ln1_b"][layer]),
                ln2g=colvec(inp["ln2_g"][layer]), ln2b=colvec(inp["ln2_b"][layer]))


_PROGS = {}


def _prog(name, builder):
    if name not in _PROGS:
        _PROGS[name] = builder()
    return _PROGS[name]


def _run(nc, maps):
    res = run_bass_kernel_spmd(nc, maps, core_ids=list(range(8)))
    return res.results


def kernel(x, attn_w_in, attn_b_in, attn_sinks, attn_w_out, mlstm_w_in, mlstm_b_in, mlstm_w_out,
           ffn_w_up, ffn_conv_w, ffn_conv_b, ffn_w_down, ln1_g, ln1_b, ln2_g, ln2_b):
    inp = dict(x=x, attn_w_in=attn_w_in, attn_b_in=attn_b_in, attn_sinks=attn_sinks, attn_w_out=attn_w_out,
               mlstm_w_in=mlstm_w_in, mlstm_b_in=mlstm_b_in, mlstm_w_out=mlstm_w_out, ffn_w_up=ffn_w_up,
               ffn_conv_w=ffn_conv_w, ffn_conv_b=ffn_conv_b, ffn_w_down=ffn_w_down, ln1_g=ln1_g, ln1_b=ln1_b,
               ln2_g=ln2_g, ln2_b=ln2_b)
    inp = {k: np.asarray(v, dtype=np.float32) for k, v in inp.items()}
    cst = make_consts()
    rope = make_rope()
    xT = [np.ascontiguousarray(inp["x"][b].T) for b in range(4)]
    xsrc = [np.ascontiguousarray(xT[b].reshape(D, 2, TH).transpose(1, 0, 2)) for b in range(4)]
    xres = []
    for c in range(8):
        b, hh = c // 2, c % 2
        xr = np.zeros((D, NH), np.float32)
        xr[:, 2:] = xT[b][:, TH * hh:TH * (hh + 1)]
        if hh == 1:
            xr[:, 0:2] = xT[b][:, TH - 2:TH]
        xres.append(xr)
    flags = [np.full((128, 1), float(c % 2), np.float32) for c in range(8)]
    for layer in range(4):
        j = layer // 2
        even = layer % 2 == 0
        maps = []
        for c in range(8):
            b, hh = c // 2, c % 2
            if even:
                m = even_inputs(inp["attn_w_in"][j], inp["attn_b_in"][j], inp["attn_sinks"][j], hh)
                m.update(cst=cst, rope=rope)
            else:
                m = odd_inputs(inp["mlstm_w_in"][j], inp["mlstm_b_in"][j], hh)
                m.update(cst=cst)
            m["xsrc"] = xsrc[b]
            maps.append(m)
        if even:
            nc = _prog("A_even_f32" if layer == 0 else "A_even_bf16", (lambda: build_A_even(True)) if layer == 0 else (lambda: build_A_even(False)))
        else:
            nc = _prog("A_odd", build_A_odd)
        resA = _run(nc, maps)
        wB = b_weight_inputs(inp, layer)
        maps = []
        for c in range(8):
            b, hh = c // 2, c % 2
            ho_recv = np.ascontiguousarray(np.stack([resA[2 * b + r]["ho"][hh] for r in range(2)], 0))
            m = dict(wB)
            m.update(ho_recv=ho_recv, xres=xres[c], cst=cst, flag=flags[c])
            maps.append(m)
        resB = _run(_prog("B", build_B), maps)
        xsrc = [np.ascontiguousarray(np.stack([resB[2 * b + r]["xn_out"] for r in range(2)], 0)) for b in range(4)]
        xres = []
        for c in range(8):
            b, hh = c // 2, c % 2
            xr = np.zeros((D, NH), np.float32)
            xr[:, 2:] = resB[c]["xT_out"]
            if hh == 1:
                xr[:, 0:2] = resB[c - 1]["xT_out"][:, TH - 2:TH]
            xres.append(xr)
    out = np.zeros((4, T, D), np.float32)
    for c in range(8):
        b, hh = c // 2, c % 2
        out[b, TH * hh:TH * (hh + 1), :] = resB[c]["xT_out"].T
    return out
```
